# Optimizing a Trainium2 kernel written in Bass

```python
import math, functools
import jax, jax.numpy as jnp
from jax import lax
import numpy as np

D_MODEL = 2048
BATCH = 4
SEQ = 2048
DEPTH = 2

GRID_W = 64
CTX_LEN = 256
EPS = 1e-6
ROPE_BASE = 10000.0
Q_BLOCK = 128
D_FF = 4 * D_MODEL

HEAD_DIM = 128
MIX_HEADS = D_MODEL // HEAD_DIM
A_HEADS = MIX_HEADS // 2
A_SUB = HEAD_DIM // 2
B_HEADS = MIX_HEADS - A_HEADS
B_DK = HEAD_DIM
B_DV = HEAD_DIM
CONV_K = 5
CHUNK = 64
A_QK = A_HEADS * 2 * A_SUB
A_V = A_HEADS * HEAD_DIM
B_QK = B_HEADS * B_DK
B_V = B_HEADS * B_DV
EVEN_SPLITS = (A_QK, A_QK, A_V, B_QK, B_QK, B_V, B_V, B_HEADS, B_HEADS, B_HEADS, B_HEADS)
EVEN_IN = sum(EVEN_SPLITS)

QK_NOPE = 128
QK_ROPE = 64
V_HEAD = 128
C_HEADS = D_MODEL // V_HEAD
Q_LORA = 512
KV_LORA = 512
ODD_IN = Q_LORA + KV_LORA + QK_ROPE

kernel_name = 'hybrid_diffattn_gdn_mla_prefix_dit'


def rms_norm(x, gain):
    xf = x.astype(jnp.float32)
    y = xf * lax.rsqrt(jnp.mean(xf * xf, axis=-1, keepdims=True) + EPS)
    return (y * gain.astype(jnp.float32)).astype(x.dtype)


def l2_norm(x):
    xf = x.astype(jnp.float32)
    return (xf * lax.rsqrt(jnp.sum(xf * xf, axis=-1, keepdims=True) + EPS)).astype(x.dtype)


def split_heads(t, n_heads):
    b, l, hd = t.shape
    return t.reshape(b, l, n_heads, hd // n_heads).transpose(0, 2, 1, 3)


def merge_heads(t):
    b, h, l, d = t.shape
    return t.transpose(0, 2, 1, 3).reshape(b, l, h * d)


def axial_rope_tables(n_tokens, rot_dim):
    rows = n_tokens // GRID_W
    t = jnp.arange(rows * GRID_W)
    row = (t // GRID_W).astype(jnp.float32)
    col = (t % GRID_W).astype(jnp.float32)
    n_freq = rot_dim // 4
    inv_freq = ROPE_BASE ** (-jnp.arange(n_freq, dtype=jnp.float32) / n_freq)
    ang = jnp.concatenate([row[:, None] * inv_freq, col[:, None] * inv_freq], axis=-1)
    return jnp.cos(ang), jnp.sin(ang)


def apply_rope(x, cos, sin):
    half = x.shape[-1] // 2
    x1, x2 = x[..., :half], x[..., half:]
    cos = cos.astype(x.dtype)
    sin = sin.astype(x.dtype)
    return jnp.concatenate([x1 * cos - x2 * sin, x2 * cos + x1 * sin], axis=-1)


def sweep_query_blocks(fn, *qs):
    b, h, s, _ = qs[0].shape
    nb = s // Q_BLOCK
    blocks = tuple(jnp.moveaxis(q.reshape(b, h, nb, Q_BLOCK, q.shape[-1]), 2, 0) for q in qs)
    out = lax.map(lambda qb: fn(*qb), blocks)
    return jnp.moveaxis(out, 0, 2).reshape(b, h, s, out.shape[-1])


def sq_relu_mlp(h, w1, w2):
    return jnp.square(jax.nn.relu(h @ w1)) @ w2


def diff_qk(t, gain):
    b, l, _ = t.shape
    t = rms_norm(t.reshape(b, l, A_HEADS, 2, A_SUB), gain).transpose(0, 2, 3, 1, 4)
    return t[:, :, 0], t[:, :, 1]


def diff_attn_core(q1, q2, k1, k2, v, lam):
    scale = A_SUB ** -0.5
    p1 = jax.nn.softmax(jnp.einsum('bhqd,bhkd->bhqk', q1, k1).astype(jnp.float32) * scale, axis=-1)
    p2 = jax.nn.softmax(jnp.einsum('bhqd,bhkd->bhqk', q2, k2).astype(jnp.float32) * scale, axis=-1)
    return jnp.einsum('bhqk,bhkd->bhqd', (p1 - lam * p2).astype(v.dtype), v)


def short_conv(x, w):
    y = lax.conv_general_dilated(x, w[:, None, :], window_strides=(1,),
                                 padding=[(CONV_K // 2, CONV_K // 2)],
                                 dimension_numbers=('NWC', 'WIO', 'NWC'),
                                 feature_group_count=x.shape[-1])
    return jax.nn.silu(y)


def gdn_gates(a, bt, alog, dtb):
    f32 = jnp.float32
    g = -jnp.exp(alog.astype(f32)) * jax.nn.softplus(a.astype(f32) + dtb.astype(f32))
    beta = jax.nn.sigmoid(bt.astype(f32))
    return jnp.swapaxes(beta, 1, 2), jnp.swapaxes(g, 1, 2)


def unit_lower_inverse(a):
    n = a.shape[-1]
    p = -a
    t = jnp.eye(n, dtype=a.dtype) + p
    for _ in range(int(math.log2(n)) - 1):
        p = p @ p
        t = t + t @ p
    return t


def gdn_chunked(q, k, v, beta, g, state):
    out_dtype = v.dtype
    f32 = jnp.float32
    q, k, v, beta, g, state = (t.astype(f32) for t in (q, k, v, beta, g, state))
    b, h, l, dk = q.shape
    dv = v.shape[-1]
    n = l // CHUNK
    ch = lambda t: t.reshape(b, h, n, CHUNK, *t.shape[3:])
    q, k, v, beta, g = ch(q), ch(k), ch(v), ch(beta), ch(g)
    G = jnp.cumsum(g, axis=-1)
    idx = jnp.arange(CHUNK)
    incl = idx[:, None] >= idx[None, :]
    strict = idx[:, None] > idx[None, :]
    gamma = jnp.exp(jnp.where(incl, G[..., :, None] - G[..., None, :], -jnp.inf))
    a_mat = jnp.where(strict, beta[..., :, None] * jnp.einsum('bhncd,bhned->bhnce', k, k) * gamma, 0.0)
    t_inv = unit_lower_inverse(a_mat)
    w = t_inv @ (k * (beta * jnp.exp(G))[..., None])
    u = t_inv @ (v * beta[..., None])
    qk = jnp.einsum('bhncd,bhned->bhnce', q, k) * gamma
    q_dec = q * jnp.exp(G)[..., None]
    k_dec = k * jnp.exp(G[..., -1:] - G)[..., None]
    g_tot = jnp.exp(G[..., -1])

    def step(s, xs):
        w_i, u_i, qk_i, qd_i, kd_i, gt_i = xs
        v_new = u_i - w_i @ s
        o_i = qd_i @ s + qk_i @ v_new
        s = gt_i[..., None, None] * s + jnp.swapaxes(kd_i, -1, -2) @ v_new
        return s, o_i

    xs = tuple(jnp.moveaxis(t, 2, 0) for t in (w, u, qk, q_dec, k_dec, g_tot))
    state, o = lax.scan(step, state, xs)
    o = jnp.moveaxis(o, 0, 2).reshape(b, h, l, dv)
    return o.astype(out_dtype), state


def gdn_bidirectional(q, k, v, beta_f, g_f, beta_b, g_b, s0_f, s0_b):
    o_f, s_f = gdn_chunked(q, k, v, beta_f, g_f, s0_f)
    rev = lambda t: jnp.flip(t, axis=2)
    o_b, s_b = gdn_chunked(rev(q), rev(k), rev(v), rev(beta_b), rev(g_b), s0_b)
    return o_f + rev(o_b), s_f, s_b


def even_mixer(h, hc, want_ctx, P, cos, sin, lam_init):
    cuts = [int(s) for s in np.cumsum(EVEN_SPLITS)[:-1]]
    aq, ak, av, bq, bk, bv, bz, af, ab, bf, bb = jnp.split(h @ P['w_in'], cuts, axis=-1)
    caq, cak, cav, cbq, cbk, cbv, cbz, caf, cab, cbf, cbb = jnp.split(hc @ P['w_in'], cuts, axis=-1)
    f32 = jnp.float32
    lam = (jnp.exp(jnp.sum(P['lam_q1'].astype(f32) * P['lam_k1'].astype(f32)))
           - jnp.exp(jnp.sum(P['lam_q2'].astype(f32) * P['lam_k2'].astype(f32))) + lam_init)

    q1, q2 = diff_qk(aq, P['q_norm'])
    q1, q2 = apply_rope(q1, cos, sin), apply_rope(q2, cos, sin)
    k1, k2 = diff_qk(ak, P['k_norm'])
    k1, k2 = apply_rope(k1, cos, sin), apply_rope(k2, cos, sin)
    ck1, ck2 = diff_qk(cak, P['k_norm'])
    cv = split_heads(cav, A_HEADS)
    k1_all = jnp.concatenate([ck1, k1], axis=2)
    k2_all = jnp.concatenate([ck2, k2], axis=2)
    v_all = jnp.concatenate([cv, split_heads(av, A_HEADS)], axis=2)
    o_a = sweep_query_blocks(lambda qa, qb: diff_attn_core(qa, qb, k1_all, k2_all, v_all, lam), q1, q2)
    finish_a = lambda o: merge_heads(rms_norm(o, P['subln']) * (1.0 - lam_init))

    def gdn_prepare(xq, xk, xv, a_f, a_b, b_f, b_b):
        qkv = short_conv(jnp.concatenate([xq, xk, xv], axis=-1), P['conv'])
        q, k, v = jnp.split(qkv, [B_QK, 2 * B_QK], axis=-1)
        q = l2_norm(split_heads(q, B_HEADS)) * (B_DK ** -0.5)
        k = l2_norm(split_heads(k, B_HEADS))
        v = split_heads(v, B_HEADS)
        beta_f, g_f = gdn_gates(a_f, b_f, P['alog_f'], P['dtb_f'])
        beta_b, g_b = gdn_gates(a_b, b_b, P['alog_b'], P['dtb_b'])
        return q, k, v, beta_f, g_f, beta_b, g_b

    zero = jnp.zeros((h.shape[0], B_HEADS, B_DK, B_DV), jnp.float32)
    oc_b, s_f, s_b = gdn_bidirectional(*gdn_prepare(cbq, cbk, cbv, caf, cab, cbf, cbb), zero, zero)
    o_b, _, _ = gdn_bidirectional(*gdn_prepare(bq, bk, bv, af, ab, bf, bb), s_f, s_b)
    finish_b = lambda o, z: merge_heads(rms_norm(o, P['o_norm']) * jax.nn.silu(split_heads(z, B_HEADS)))

    y = jnp.concatenate([finish_a(o_a), finish_b(o_b, bz)], axis=-1) @ P['w_out']
    yc = None
    if want_ctx:
        cq1, cq2 = diff_qk(caq, P['q_norm'])
        oc_a = diff_attn_core(cq1, cq2, ck1, ck2, cv, lam)
        yc = jnp.concatenate([finish_a(oc_a), finish_b(oc_b, cbz)], axis=-1) @ P['w_out']
    return y, yc


def mla_core(q_nope, q_rope, k_nope, k_rope, v):
    s = (jnp.einsum('bhqd,bhkd->bhqk', q_nope, k_nope)
         + jnp.einsum('bhqr,bkr->bhqk', q_rope, k_rope))
    p = jax.nn.softmax(s.astype(jnp.float32) * ((QK_NOPE + QK_ROPE) ** -0.5), axis=-1)
    return jnp.einsum('bhqk,bhkd->bhqd', p.astype(v.dtype), v)


def odd_mixer(h, hc, want_ctx, P, cos, sin):
    def keys(proj, rope):
        ckv = proj[..., Q_LORA:Q_LORA + KV_LORA]
        kv = split_heads(rms_norm(ckv, P['kva_norm']) @ P['w_ukv'], C_HEADS)
        k_nope = rms_norm(kv[..., :QK_NOPE], P['kn_norm'])
        v = kv[..., QK_NOPE:]
        k_rope = rms_norm(proj[..., Q_LORA + KV_LORA:], P['kr_norm'])
        if rope:
            k_rope = apply_rope(k_rope, cos, sin)
        return k_nope, k_rope, v

    def queries(proj, rope):
        q = split_heads(rms_norm(proj[..., :Q_LORA], P['qa_norm']) @ P['w_uq'], C_HEADS)
        q_nope = rms_norm(q[..., :QK_NOPE], P['qn_norm'])
        q_rope = rms_norm(q[..., QK_NOPE:], P['qr_norm'])
        if rope:
            q_rope = apply_rope(q_rope, cos, sin)
        return q_nope, q_rope

    proj = h @ P['w_in']
    proj_c = hc @ P['w_in']
    ck_nope, ck_rope, cv = keys(proj_c, False)
    k_nope, k_rope, v = keys(proj, True)
    k_nope_all = jnp.concatenate([ck_nope, k_nope], axis=2)
    k_rope_all = jnp.concatenate([ck_rope, k_rope], axis=1)
    v_all = jnp.concatenate([cv, v], axis=2)
    q_nope, q_rope = queries(proj, True)
    o = sweep_query_blocks(lambda qn, qr: mla_core(qn, qr, k_nope_all, k_rope_all, v_all), q_nope, q_rope)
    y = merge_heads(o) @ P['w_out']
    yc = None
    if want_ctx:
        cq_nope, cq_rope = queries(proj_c, False)
        yc = merge_heads(mla_core(cq_nope, cq_rope, ck_nope, ck_rope, cv)) @ P['w_out']
    return y, yc


def residual_block(x, xc, mod, mod_c, n1, n2, w1, w2, mixer, want_ctx):
    sh1, sc1, g1, sh2, sc2, g2 = jnp.split(mod[:, None, :], 6, axis=-1)
    csh1, csc1, cg1, csh2, csc2, cg2 = jnp.split(mod_c[:, None, :], 6, axis=-1)
    y, yc = mixer(rms_norm(x, n1) * (1.0 + sc1) + sh1, rms_norm(xc, n1) * (1.0 + csc1) + csh1, want_ctx)
    x = x + g1 * y
    x = x + g2 * sq_relu_mlp(rms_norm(x, n2) * (1.0 + sc2) + sh2, w1, w2)
    if want_ctx:
        xc = xc + cg1 * yc
        xc = xc + cg2 * sq_relu_mlp(rms_norm(xc, n2) * (1.0 + csc2) + csh2, w1, w2)
    return x, xc


def setup_inputs(seed: int = 0) -> dict:
    key = jax.random.key(seed)
    ks = iter(jax.random.split(key, 48))
    nrm = lambda shape, std: jax.random.normal(next(ks), shape, jnp.float32) * std
    gain = lambda shape: 1.0 + nrm(shape, 0.05)
    d = D_MODEL
    ne, no = (DEPTH + 1) // 2, DEPTH // 2
    col_scale = jnp.concatenate([
        jnp.full((EVEN_IN - 4 * B_HEADS,), d ** -0.5, jnp.float32),
        jnp.full((2 * B_HEADS,), 0.1 * d ** -0.5, jnp.float32),
        jnp.full((2 * B_HEADS,), d ** -0.5, jnp.float32)])

    def alog():
        return jnp.log(jax.random.uniform(next(ks), (ne, B_HEADS), jnp.float32, 1.0, 16.0))

    def dt_bias():
        dt = jnp.exp(jax.random.uniform(next(ks), (ne, B_HEADS), jnp.float32, math.log(1e-3), math.log(1e-1)))
        return dt + jnp.log(-jnp.expm1(-dt))

    return {
        'x': nrm((BATCH, SEQ, d), 1.0),
        'c': nrm((BATCH, d), 1.0),
        'ctx': nrm((BATCH, CTX_LEN, d), 1.0),
        'c_ctx': nrm((d,), 1.0),
        'ada_w': nrm((DEPTH, d, 6 * d), 0.5 * d ** -0.5),
        'ada_b': nrm((DEPTH, 6 * d), 0.02),
        'norm1': gain((DEPTH, d)),
        'norm2': gain((DEPTH, d)),
        'mlp_w1': nrm((DEPTH, d, D_FF), d ** -0.5),
        'mlp_w2': nrm((DEPTH, D_FF, d), D_FF ** -0.5),
        'e_w_in': nrm((ne, d, EVEN_IN), 1.0) * col_scale,
        'e_q_norm': gain((ne, A_SUB)),
        'e_k_norm': gain((ne, A_SUB)),
        'e_lam_q1': nrm((ne, A_SUB), 0.1),
        'e_lam_k1': nrm((ne, A_SUB), 0.1),
        'e_lam_q2': nrm((ne, A_SUB), 0.1),
        'e_lam_k2': nrm((ne, A_SUB), 0.1),
        'e_subln': gain((ne, HEAD_DIM)),
        'e_conv': nrm((ne, CONV_K, 2 * B_QK + B_V), CONV_K ** -0.5),
        'e_alog_f': alog(),
        'e_alog_b': alog(),
        'e_dtb_f': dt_bias(),
        'e_dtb_b': dt_bias(),
        'e_o_norm': gain((ne, B_DV)),
        'e_w_out': nrm((ne, A_V + B_V, d), (A_V + B_V) ** -0.5),
        'o_w_in': nrm((no, d, ODD_IN), d ** -0.5),
        'o_qa_norm': gain((no, Q_LORA)),
        'o_w_uq': nrm((no, Q_LORA, C_HEADS * (QK_NOPE + QK_ROPE)), Q_LORA ** -0.5),
        'o_kva_norm': gain((no, KV_LORA)),
        'o_w_ukv': nrm((no, KV_LORA, C_HEADS * (QK_NOPE + V_HEAD)), KV_LORA ** -0.5),
        'o_qn_norm': gain((no, QK_NOPE)),
        'o_qr_norm': gain((no, QK_ROPE)),
        'o_kn_norm': gain((no, QK_NOPE)),
        'o_kr_norm': gain((no, QK_ROPE)),
        'o_w_out': nrm((no, C_HEADS * V_HEAD, d), (C_HEADS * V_HEAD) ** -0.5),
    }


def reference(x, c, ctx, c_ctx, ada_w, ada_b, norm1, norm2, mlp_w1, mlp_w2,
              e_w_in, e_q_norm, e_k_norm, e_lam_q1, e_lam_k1, e_lam_q2, e_lam_k2, e_subln, e_conv,
              e_alog_f, e_alog_b, e_dtb_f, e_dtb_b, e_o_norm, e_w_out,
              o_w_in, o_qa_norm, o_w_uq, o_kva_norm, o_w_ukv, o_qn_norm, o_qr_norm, o_kn_norm, o_kr_norm,
              o_w_out):
    n_lat = x.shape[1]
    cos_a, sin_a = axial_rope_tables(n_lat, A_SUB)
    cos_c, sin_c = axial_rope_tables(n_lat, QK_ROPE)
    silu_c = jax.nn.silu(c)
    silu_cc = jax.nn.silu(c_ctx)[None, :]
    xc = ctx
    for i in range(DEPTH):
        want_ctx = i < DEPTH - 1
        j = i // 2
        mod = silu_c @ ada_w[i] + ada_b[i]
        mod_c = silu_cc @ ada_w[i] + ada_b[i]
        if i % 2 == 0:
            P = dict(w_in=e_w_in[j], q_norm=e_q_norm[j], k_norm=e_k_norm[j],
                     lam_q1=e_lam_q1[j], lam_k1=e_lam_k1[j], lam_q2=e_lam_q2[j], lam_k2=e_lam_k2[j],
                     subln=e_subln[j], conv=e_conv[j], alog_f=e_alog_f[j], alog_b=e_alog_b[j],
                     dtb_f=e_dtb_f[j], dtb_b=e_dtb_b[j], o_norm=e_o_norm[j], w_out=e_w_out[j])
            mixer = functools.partial(even_mixer, P=P, cos=cos_a, sin=sin_a,
                                      lam_init=0.8 - 0.6 * math.exp(-0.3 * i))
        else:
            P = dict(w_in=o_w_in[j], qa_norm=o_qa_norm[j], w_uq=o_w_uq[j], kva_norm=o_kva_norm[j],
                     w_ukv=o_w_ukv[j], qn_norm=o_qn_norm[j], qr_norm=o_qr_norm[j],
                     kn_norm=o_kn_norm[j], kr_norm=o_kr_norm[j], w_out=o_w_out[j])
            mixer = functools.partial(odd_mixer, P=P, cos=cos_c, sin=sin_c)
        x, xc = residual_block(x, xc, mod, mod_c, norm1[i], norm2[i], mlp_w1[i], mlp_w2[i], mixer, want_ctx)
    return x
```

```python
import math
from contextlib import ExitStack
import numpy as np
import concourse.bass as bass
import concourse.mybir as mybir
from concourse.alu_op_type import AluOpType as ALU
from concourse.bass_utils import run_bass_kernel_spmd

F32 = mybir.dt.float32
F32R = mybir.dt.float32r
AF = mybir.ActivationFunctionType
AX = mybir.AxisListType
NCORES = 8


class Tile:
    def __init__(self, kb, name, h, space):
        self.kb = kb
        self.name = name
        self.h = h
        self.space = space
        self.last_w = None
        self.readers = []
        self.sem = None
        self.dcount = 0

    def __getitem__(self, idx):
        return View(self, self.h[idx])

    def v(self):
        return View(self, self.h[:])

    def re(self, fn):
        return self.v().re(fn)

    def r(self):
        return self.v().r()


class View:
    def __init__(self, t, ap):
        self.t = t
        self.ap = ap

    def __getitem__(self, idx):
        return View(self.t, self.ap[idx])

    def re(self, fn):
        return View(self.t, fn(self.ap))

    def r(self):
        return View(self.t, self.ap.bitcast(F32R))


def _v(x):
    return x.v() if isinstance(x, Tile) else x


ENGS = ("pe", "act", "dve", "pool", "sp")


class KB:
    def __init__(self):
        self.nc = bass.Bass("TRN2", target_bir_lowering=False)
        self.es = ExitStack()
        self.q = {e: [] for e in ENGS}
        self.cnt = {e: 0 for e in ENGS}
        self.seen = {e: {} for e in ENGS}
        self.esem = {}
        for e in ("pe", "act", "dve", "pool"):
            self.esem[e] = self.es.enter_context(self.nc.semaphore("es_" + e))
        self.nsem = 4
        self.tiles = []
        self.n_inst = 0

    def dram(self, name, shape, kind):
        return self.nc.dram_tensor(name, list(shape), F32, kind=kind).ap()

    def sb(self, name, shape, dtype=F32):
        h = self.es.enter_context(self.nc.sbuf_tensor(name, list(shape), dtype))
        t = Tile(self, name, h, "sbuf")
        self.tiles.append(t)
        return t

    def ps(self, name, shape, dtype=F32):
        h = self.es.enter_context(self.nc.psum_tensor(name, list(shape), dtype))
        t = Tile(self, name, h, "psum")
        self.tiles.append(t)
        return t

    def _need(self, eng, ev):
        if ev is None:
            return
        if ev[0] == "e":
            _, src, n = ev
            key = ("e", src)
            sem = self.esem[src]
            val = n
        else:
            _, tile, n = ev
            key = ("d", id(tile))
            sem = tile.sem
            val = n
        if self.seen[eng].get(key, 0) >= val:
            return
        self.seen[eng][key] = val
        self.q[eng].append(lambda e, sem=sem, val=val: e.wait_ge(sem, val))

    def _deps(self, eng, reads, writes):
        for t in reads:
            ev = t.last_w
            if ev is not None and not (ev[0] == "e" and ev[1] == eng and eng == "pe"):
                self._need(eng, ev)
        for t in writes:
            ev = t.last_w
            if ev is not None and not (ev[0] == "e" and ev[1] == eng and eng == "pe"):
                self._need(eng, ev)
            for rv in t.readers:
                if rv[0] == "e" and rv[1] == eng:
                    continue
                self._need(eng, rv)

    def op(self, eng, build, reads, writes, sig=True):
        reads = [x.t for x in reads if x is not None and not isinstance(x, (int, float))]
        writes = [x.t for x in writes]
        self._deps(eng, reads, writes)
        self.n_inst += 1
        if sig:
            self.cnt[eng] += 1
            n = self.cnt[eng]
            sem = self.esem[eng]
            self.q[eng].append(lambda e, build=build, sem=sem: build(e).then_inc(sem, 1))
        else:
            n = self.cnt[eng] + 1
            self.q[eng].append(lambda e, build=build: build(e))
        ev = ("e", eng, n)
        for t in reads:
            t.readers.append(ev)
        for t in writes:
            t.last_w = ev
            t.readers = []

    def dma(self, q, out, in_, **kw):
        if isinstance(out, (View, Tile)):
            out = _v(out)
            tile, load = out.t, True
            o_ap, i_ap = out.ap, in_
        else:
            in_ = _v(in_)
            tile, load = in_.t, False
            o_ap, i_ap = out, in_.ap
        if tile.sem is None:
            tile.sem = self.es.enter_context(self.nc.semaphore("ds_%d" % self.nsem))
            self.nsem += 1
            assert self.nsem < 230, "too many semaphores"
        if load:
            self._deps(q, [], [tile])
        else:
            self._deps(q, [tile], [])
        tile.dcount += 1
        ev = ("d", tile, 16 * tile.dcount)
        sem = tile.sem
        self.q[q].append(lambda e, o=o_ap, i=i_ap, sem=sem, kw=kw: e.dma_start(out=o, in_=i, **kw).then_inc(sem, 16))
        self.n_inst += 1
        if load:
            tile.last_w = ev
            tile.readers = []
        else:
            tile.readers.append(ev)

    def mm(self, out, lhsT, rhs, start=True, stop=True, r=True):
        out, lhsT, rhs = _v(out), _v(lhsT), _v(rhs)
        la = lhsT.ap.bitcast(F32R) if r else lhsT.ap
        ra = rhs.ap.bitcast(F32R) if r else rhs.ap
        oa = out.ap
        self.op("pe", lambda e: e.matmul(oa, la, ra, start=start, stop=stop), [lhsT, rhs], [out], sig=stop)

    def tr(self, out, in_, ident):
        out, in_, ident = _v(out), _v(in_), _v(ident)
        oa, ia, da = out.ap, in_.ap, ident.ap
        self.op("pe", lambda e: e.transpose(oa, ia, da), [in_, ident], [out])

    def act(self, out, in_, func, bias=None, scale=None, accum=None, eng="act"):
        out, in_ = _v(out), _v(in_)
        kw = {}
        rd = [in_]
        if bias is not None:
            if isinstance(bias, (int, float)):
                kw["bias"] = float(bias)
            else:
                bias = _v(bias)
                kw["bias"] = bias.ap
                rd.append(bias)
        if scale is not None:
            if isinstance(scale, (int, float)):
                kw["scale"] = float(scale)
            else:
                scale = _v(scale)
                kw["scale"] = scale.ap
                rd.append(scale)
        wr = [out]
        if accum is not None:
            accum = _v(accum)
            kw["accum_out"] = accum.ap
            wr.append(accum)
        oa, ia = out.ap, in_.ap
        self.op("act", lambda e: e.activation(oa, ia, func, **kw), rd, wr)

    def tt(self, eng, out, in0, in1, op):
        out, in0, in1 = _v(out), _v(in0), _v(in1)
        oa, a, b = out.ap, in0.ap, in1.ap
        self.op(eng, lambda e: e.tensor_tensor(oa, a, b, op), [in0, in1], [out])

    def ts(self, eng, out, in0, s1, op0, s2=None, op1=None, accum=None):
        out, in0 = _v(out), _v(in0)
        rd = [in0]

        def cv(s):
            if s is None or isinstance(s, (int, float)):
                return None if s is None else float(s)
            s = _v(s)
            rd.append(s)
            return s.ap

        a1, a2 = cv(s1), cv(s2)
        wr = [out]
        kw = {}
        if accum is not None:
            accum = _v(accum)
            kw["accum_out"] = accum.ap
            wr.append(accum)
        oa, ia = out.ap, in0.ap
        if op1 is None:
            self.op(eng, lambda e: e.tensor_scalar(oa, ia, a1, None, op0, **kw), rd, wr)
        else:
            self.op(eng, lambda e: e.tensor_scalar(oa, ia, a1, a2, op0, op1, **kw), rd, wr)

    def stt(self, out, in0, scalar, in1, op0, op1, eng="dve"):
        out, in0, in1 = _v(out), _v(in0), _v(in1)
        rd = [in0, in1]
        if isinstance(scalar, (int, float)):
            sa = float(scalar)
        else:
            scalar = _v(scalar)
            sa = scalar.ap
            rd.append(scalar)
        oa, a, b = out.ap, in0.ap, in1.ap
        self.op(eng, lambda e: e.scalar_tensor_tensor(oa, a, sa, b, op0, op1), rd, [out])

    def copy(self, eng, out, in_):
        out, in_ = _v(out), _v(in_)
        oa, ia = out.ap, in_.ap
        if eng == "act":
            self.op("act", lambda e: e.copy(oa, ia), [in_], [out])
        else:
            self.op(eng, lambda e: e.tensor_copy(oa, ia), [in_], [out])

    def recip(self, out, in_):
        out, in_ = _v(out), _v(in_)
        oa, ia = out.ap, in_.ap
        self.op("dve", lambda e: e.reciprocal(oa, ia), [in_], [out])

    def reduce(self, out, in_, op, axis=AX.X):
        out, in_ = _v(out), _v(in_)
        oa, ia = out.ap, in_.ap
        self.op("dve", lambda e: e.tensor_reduce(oa, ia, axis, op), [in_], [out])

    def memset(self, eng, out, val):
        out = _v(out)
        oa = out.ap
        self.op(eng, lambda e: e.memset(oa, float(val)), [], [out])

    def build(self):
        for t in self.tiles:
            if t.sem is not None and t.dcount > 0:
                self._need("sp", ("d", t, 16 * t.dcount))
        nc = self.nc
        with nc.Block() as block:
            @block.tensor
            def _(e):
                for f in self.q["pe"]:
                    f(e)

            @block.scalar
            def _(e):
                for f in self.q["act"]:
                    f(e)

            @block.vector
            def _(e):
                for f in self.q["dve"]:
                    f(e)

            @block.gpsimd
            def _(e):
                for f in self.q["pool"]:
                    f(e)

            @block.sync
            def _(e):
                for f in self.q["sp"]:
                    f(e)
        self.es.close()
        return nc


def run(kb_nc, in_maps):
    res = run_bass_kernel_spmd(kb_nc, in_maps, core_ids=list(range(NCORES)))
    return res.results


EPS = 1e-6
D = 2048


class Ctx:
    def __init__(self, kb, nw=2, ident_ap=None):
        self.kb = kb
        self.pp_tiles = [kb.ps("pp%d" % i, [128, 512]) for i in range(8)]
        self.pi = 0
        self.wb = [kb.sb("wb%d" % i, [128, 8192]) for i in range(nw)]
        self.wi = 0
        self.qi = 0
        if ident_ap is not None:
            self.ident = kb.sb("ident", [128, 128])
            kb.dma("sp", self.ident, ident_ap)

    def pp(self):
        t = self.pp_tiles[self.pi % len(self.pp_tiles)]
        self.pi += 1
        return t

    def wload(self, w_ap, k0, kc, n0, ncols):
        t = self.wb[self.wi % len(self.wb)]
        self.wi += 1
        v = t[:, 0:kc * ncols].re(lambda a: a.rearrange("p (c n) -> p c n", c=kc))
        src = w_ap.bitcast(F32R)[k0:k0 + kc * 128, n0:n0 + ncols].rearrange("(c p) n -> p c n", p=128)
        self.kb.dma("pool", v.r(), src)
        return v


def norm_rows(kb, x, A, B, h_out, ss, rt, rstd, nfeat):
    kb.act(h_out, x, AF.Square, accum=ss)
    kb.act(rt, ss, AF.Sqrt, scale=1.0 / nfeat, bias=EPS)
    kb.recip(rstd, rt)
    kb.stt(h_out, x, rstd, A, ALU.mult, ALU.mult)
    if B is not None:
        kb.tt("pool", h_out, h_out, B, ALU.add)


def transpose_rows(kb, cx, h, hT, nchunks, tcol):
    for j0 in range(0, nchunks, 4):
        n = min(4, nchunks - j0)
        ps = cx.pp()
        for j in range(n):
            kb.tr(ps[:, j * 128:(j + 1) * 128], h[:, (j0 + j) * 128:(j0 + j + 1) * 128], cx.ident)
        src = ps[:, 0:n * 128].re(lambda a: a.rearrange("p (c n) -> p c n", c=n))
        dst = hT[:, j0:j0 + n, tcol:tcol + 128].r()
        kb.copy("dve" if (j0 // 4) % 2 == 0 else "act", dst, src)


def small_scratch(kb):
    return kb.sb("ss", [128, 1]), kb.sb("rt", [128, 1]), kb.sb("rstd", [128, 1])


def build_mod():
    kb = KB()
    cT = kb.dram("cT", [128, 16, 8], "ExternalInput")
    w = kb.dram("w", [2048, 3072], "ExternalInput")
    b = kb.dram("b", [8, 3072], "ExternalInput")
    y = kb.dram("y", [8, 3072], "ExternalOutput")
    cx = Ctx(kb)
    ct = kb.sb("ct", [128, 16, 8])
    kb.dma("sp", ct, cT)
    st = kb.sb("st", [128, 16, 8])
    kb.act(st.v().r(), ct, AF.Silu)
    bt = kb.sb("bt", [8, 3072])
    kb.dma("sp", bt, b)
    yt = kb.sb("yt", [8, 3072])
    for nb in range(6):
        W = cx.wload(w, 0, 16, nb * 512, 512)
        ps = cx.pp()
        for c in range(16):
            kb.mm(ps[0:8, :], st[:, c, :], W[:, c, :], start=(c == 0), stop=(c == 15))
        kb.tt("dve", yt[:, nb * 512:(nb + 1) * 512], ps[0:8, :], bt[:, nb * 512:(nb + 1) * 512], ALU.add)
    kb.dma("sp", y, yt)
    return kb.build()


def build_pre0(T, groups, NOUT):
    kb = KB()
    x = kb.dram("x", [T, D], "ExternalInput")
    vl = kb.dram("vl", [2, 128, D], "ExternalInput")
    vc = kb.dram("vc", [2, 128, D], "ExternalInput")
    n1 = kb.dram("n1", [128, D], "ExternalInput")
    w = kb.dram("w", [D, NOUT], "ExternalInput")
    idn = kb.dram("idn", [128, 128], "ExternalInput")
    y = kb.dram("y", [T, NOUT], "ExternalOutput")
    cx = Ctx(kb, 2, idn)
    ss, rt, rstd = small_scratch(kb)
    n1t = kb.sb("n1t", [128, D])
    kb.dma("sp", n1t, n1)
    vt = kb.sb("vt", [128, 2, D])
    xg = kb.sb("xg", [128, 3, D])
    hT = kb.sb("hT", [128, 16, 384])
    htmp = kb.sb("htmp", [128, D])
    stg = [kb.sb("stg%d" % i, [128, 512]) for i in range(3)]
    si = 0
    for (t0, nt, is_ctx) in groups:
        G = nt * 128
        kb.dma("sp", xg[:, 0:nt, :], x[t0 * 128:(t0 + nt) * 128, :].rearrange("(t p) d -> p t d", p=128))
        kb.dma("sp", vt, (vc if is_ctx else vl).rearrange("v p d -> p v d"))
        kb.stt(vt[:, 0, :], vt[:, 0, :], 1.0, n1t, ALU.add, ALU.mult)
        for t in range(nt):
            norm_rows(kb, xg[:, t, :], vt[:, 0, :], vt[:, 1, :], htmp, ss, rt, rstd, D)
            transpose_rows(kb, cx, htmp, hT, 16, t * 128)
        for n0 in range(0, NOUT, 512):
            nc_ = min(512, NOUT - n0)
            W = cx.wload(w, 0, 16, n0, nc_)
            for t in range(nt):
                ps = cx.pp()
                for c in range(16):
                    kb.mm(ps[:, 0:nc_], hT[:, c, t * 128:(t + 1) * 128], W[:, c, :], start=(c == 0), stop=(c == 15))
                s = stg[si % 3]
                kb.copy("act" if si % 2 == 0 else "dve", s[:, 0:nc_], ps[:, 0:nc_])
                si += 1
                kb.dma("sp", y[(t0 + t) * 128:(t0 + t + 1) * 128, n0:n0 + nc_], s[:, 0:nc_])
    return kb.build()


def build_post(T, groups):
    kb = KB()
    x = kb.dram("x", [T, D], "ExternalInput")
    oT = kb.dram("oT", [D, T], "ExternalInput")
    vl = kb.dram("vl", [4, 128, D], "ExternalInput")
    vc = kb.dram("vc", [4, 128, D], "ExternalInput")
    n2 = kb.dram("n2", [128, D], "ExternalInput")
    wo = kb.dram("wo", [D, D], "ExternalInput")
    w1 = kb.dram("w1", [D, 4 * D], "ExternalInput")
    w2 = kb.dram("w2", [4 * D, D], "ExternalInput")
    idn = kb.dram("idn", [128, 128], "ExternalInput")
    y = kb.dram("y", [T, D], "ExternalOutput")
    cx = Ctx(kb, 2, idn)
    ss, rt, rstd = small_scratch(kb)
    vt = kb.sb("vt", [128, 4, D])
    xg = kb.sb("xg", [128, 3, D])
    hT = kb.sb("hT", [128, 16, 384])
    hid = kb.sb("hid", [128, 16, 384])
    htmp = kb.sb("htmp", [128, D])
    tmp = [kb.sb("tmp%d" % i, [128, 512]) for i in range(2)]
    ti = 0
    for (t0, nt, is_ctx) in groups:
        G = nt * 128
        kb.dma("sp", xg[:, 0:nt, :], x[t0 * 128:(t0 + nt) * 128, :].rearrange("(t p) d -> p t d", p=128))
        kb.dma("pool", hT[:, :, 0:G].r(), oT.bitcast(F32R)[:, t0 * 128:t0 * 128 + G].rearrange("(c p) t -> p c t", p=128))
        kb.dma("sp", vt, (vc if is_ctx else vl).rearrange("v p d -> p v d"))
        kb.dma("sp", htmp, n2)
        kb.stt(vt[:, 1, :], vt[:, 1, :], 1.0, htmp, ALU.add, ALU.mult)
        for nb in range(4):
            W = cx.wload(wo, 0, 16, nb * 512, 512)
            for t in range(nt):
                ps = cx.pp()
                for c in range(16):
                    kb.mm(ps, hT[:, c, t * 128:(t + 1) * 128], W[:, c, :], start=(c == 0), stop=(c == 15))
                tm = tmp[ti % 2]
                ti += 1
                kb.tt("dve", tm, ps, vt[:, 0, nb * 512:(nb + 1) * 512], ALU.mult)
                kb.tt("pool", xg[:, t, nb * 512:(nb + 1) * 512], xg[:, t, nb * 512:(nb + 1) * 512], tm, ALU.add)
        for t in range(nt):
            norm_rows(kb, xg[:, t, :], vt[:, 1, :], vt[:, 2, :], htmp, ss, rt, rstd, D)
            transpose_rows(kb, cx, htmp, hT, 16, t * 128)
        for piece in range(4):
            for wb_ in range(4):
                W = cx.wload(w1, 0, 16, piece * 2048 + wb_ * 512, 512)
                for jj in range(4):
                    ps = cx.pp()
                    for c in range(16):
                        kb.mm(ps[:, 0:G], W[:, c, jj * 128:(jj + 1) * 128], hT[:, c, 0:G], start=(c == 0), stop=(c == 15))
                    tm = tmp[ti % 2]
                    ti += 1
                    kb.act(tm[:, 0:G], ps[:, 0:G], AF.Relu)
                    kb.tt("pool", hid[:, wb_ * 4 + jj, 0:G].r(), tm[:, 0:G], tm[:, 0:G], ALU.mult)
            for nb in range(4):
                W = cx.wload(w2, piece * 2048, 16, nb * 512, 512)
                for t in range(nt):
                    ps = cx.pp()
                    for c in range(16):
                        kb.mm(ps, hid[:, c, t * 128:(t + 1) * 128], W[:, c, :], start=(c == 0), stop=(c == 15))
                    tm = tmp[ti % 2]
                    ti += 1
                    kb.tt("dve", tm, ps, vt[:, 3, nb * 512:(nb + 1) * 512], ALU.mult)
                    kb.tt("pool", xg[:, t, nb * 512:(nb + 1) * 512], xg[:, t, nb * 512:(nb + 1) * 512], tm, ALU.add)
        kb.dma("sp", y[t0 * 128:(t0 + nt) * 128, :].rearrange("(t p) d -> p t d", p=128), xg[:, 0:nt, :])
    return kb.build()


def rep(v, n=128):
    return np.ascontiguousarray(np.broadcast_to(np.asarray(v, np.float32)[None, :], (n, v.shape[0])))


def core_tokens(lat, cx_, b, p):
    return np.ascontiguousarray(np.concatenate([cx_[b, p * 128:(p + 1) * 128], lat[b, p * 1024:(p + 1) * 1024]], 0))


def uncore_tokens(ys, ncol):
    lat = np.zeros((4, 2048, ncol), np.float32)
    cx_ = np.zeros((4, 256, ncol), np.float32)
    for c in range(8):
        b, p = c // 2, c % 2
        cx_[b, p * 128:(p + 1) * 128] = ys[c][0:128]
        lat[b, p * 1024:(p + 1) * 1024] = ys[c][128:1152]
    return lat, cx_


IDN = np.eye(128, dtype=np.float32)
GROUPS0 = [(0, 1, True), (1, 3, False), (4, 3, False), (7, 2, False)]
GROUPS1 = [(0, 3, False), (3, 3, False), (6, 2, False)]


def modvecs(mod_rows, b, idxs):
    sp = lambda row: [row[j * D:(j + 1) * D] for j in range(6)]
    vl = np.stack([rep(sp(mod_rows[b])[j]) for j in idxs])
    vc = np.stack([rep(sp(mod_rows[4])[j]) for j in idxs])
    return vl, vc


def run_mod(c, c_ctx, ada_w, ada_b):
    cc = np.zeros((8, D), np.float32)
    cc[0:4] = c
    cc[4] = c_ctx
    cT = np.ascontiguousarray(cc.T.reshape(16, 128, 8).transpose(1, 0, 2))
    wall = np.concatenate([ada_w[0], ada_w[1]], 1)
    ball = np.concatenate([ada_b[0], ada_b[1]], 0)
    ins = []
    for k in range(8):
        ins.append({"cT": cT, "w": np.ascontiguousarray(wall[:, k * 3072:(k + 1) * 3072]),
                    "b": rep(ball[k * 3072:(k + 1) * 3072], 8)})
    res = run(build_mod(), ins)
    m = np.concatenate([res[k]["y"] for k in range(8)], 1)
    return m[0:5, 0:12288], m[0:5, 12288:]


def run_pre0(x, ctx, mod0, norm1, w_in):
    ins = []
    for c in range(8):
        b, p = c // 2, c % 2
        vl, vc = modvecs(mod0, b, [1, 0])
        ins.append({"x": core_tokens(x, ctx, b, p), "vl": vl, "vc": vc, "n1": rep(norm1), "w": w_in, "idn": IDN})
    res = run(build_pre0(1152, GROUPS0, w_in.shape[1]), ins)
    return uncore_tokens([r["y"] for r in res], w_in.shape[1])


def run_post(x, ctx, o, oc, mod, norm2, wo, w1, w2, with_ctx):
    ins = []
    T = 1152 if with_ctx else 1024
    for c in range(8):
        b, p = c // 2, c % 2
        vl, vc = modvecs(mod, b, [2, 4, 3, 5])
        if with_ctx:
            xt = core_tokens(x, ctx, b, p)
            ot = core_tokens(o, oc, b, p)
        else:
            xt = np.ascontiguousarray(x[b, p * 1024:(p + 1) * 1024])
            ot = o[b, p * 1024:(p + 1) * 1024]
        ins.append({"x": xt, "oT": np.ascontiguousarray(ot.T), "vl": vl, "vc": vc, "n2": rep(norm2),
                    "wo": wo, "w1": w1, "w2": w2, "idn": IDN})
    res = run(build_post(T, GROUPS0 if with_ctx else GROUPS1), ins)
    if with_ctx:
        return uncore_tokens([r["y"] for r in res], D)
    lat = np.zeros((4, 2048, D), np.float32)
    for c in range(8):
        lat[c // 2, (c % 2) * 1024:(c % 2 + 1) * 1024] = res[c]["y"]
    return lat, None


def dma_r(kb, view, dram_ap):
    kb.dma("pool", _v(view).r(), dram_ap.bitcast(F32R))


def fm_rmsnorm(kb, cx, x, out, ones_l, gain, nfeat, tmps, P, N, rr=True):
    for n0 in range(0, N, 512):
        n = min(512, N - n0)
        sq, rt = tmps[0], tmps[1]
        kb.act(sq[0:P, 0:n].r(), x[0:P, n0:n0 + n], AF.Square)
        ps = cx.pp()
        kb.mm(ps[0:P, 0:n], ones_l, sq[0:P, 0:n], r=rr)
        kb.act(rt[0:P, 0:n], ps[0:P, 0:n], AF.Sqrt, scale=1.0 / nfeat, bias=EPS)
        kb.recip(rt[0:P, 0:n], rt[0:P, 0:n])
        ov = out[0:P, n0:n0 + n]
        kb.stt(ov.r() if rr else ov, x[0:P, n0:n0 + n], gain, rt[0:P, 0:n], ALU.mult, ALU.mult)


def fm_rope(kb, cx, xn, rotT, cosT, sinT, tmps, P, c0, N):
    for n0 in range(0, N, 512):
        n = min(512, N - n0)
        ps = cx.pp()
        kb.mm(ps[0:P, 0:n], rotT, xn[0:P, c0 + n0:c0 + n0 + n])
        t1, t2 = tmps[0], tmps[1]
        kb.tt("dve", t1[0:P, 0:n], ps[0:P, 0:n], sinT[0:P, n0:n0 + n], ALU.mult)
        kb.tt("pool", t2[0:P, 0:n], xn[0:P, c0 + n0:c0 + n0 + n], cosT[0:P, n0:n0 + n], ALU.mult)
        kb.tt("dve", xn[0:P, c0 + n0:c0 + n0 + n].r(), t1[0:P, 0:n], t2[0:P, 0:n], ALU.add)


NTOK = 2304


def build_att0(lam_init):
    kb = KB()
    NH = 4
    qT = kb.dram("qT", [NH, 128, NTOK], "ExternalInput")
    kT = kb.dram("kT", [NH, 128, NTOK], "ExternalInput")
    vv = kb.dram("v", [NH, NTOK, 128], "ExternalInput")
    cst = kb.dram("cst", [3, 128, 128], "ExternalInput")
    tabs = kb.dram("tabs", [2, 128, 2048], "ExternalInput")
    gv = kb.dram("gv", [128, 8], "ExternalInput")
    lamv = kb.dram("lamv", [128, 4, 64], "ExternalInput")
    oT = kb.dram("oT", [NH, 128, NTOK], "ExternalOutput")
    cx = Ctx(kb, 0)
    cs = kb.sb("cs", [128, 3, 128])
    dma_r(kb, cs, cst.rearrange("c p n -> p c n"))
    bones, ones, rotT = cs[:, 0, :], cs[:, 1, :], cs[:, 2, :]
    tb = kb.sb("tb", [128, 2, 2048])
    kb.dma("sp", tb, tabs.rearrange("c p n -> p c n"))
    g = kb.sb("g", [128, 8])
    kb.dma("sp", g, gv)
    kb.ts("dve", g[:, 2:3], g[:, 2:3], float(1.0 - lam_init), ALU.mult)
    lv = kb.sb("lv", [128, 4, 64])
    kb.dma("sp", lv, lamv)
    lt = kb.sb("lt", [128, 64])
    e = kb.sb("e", [128, 4])
    kb.tt("dve", lt, lv[:, 0, :], lv[:, 1, :], ALU.mult)
    kb.reduce(e[:, 0:1], lt, ALU.add)
    kb.tt("dve", lt, lv[:, 2, :], lv[:, 3, :], ALU.mult)
    kb.reduce(e[:, 1:2], lt, ALU.add)
    kb.act(e[:, 2:4], e[:, 0:2], AF.Exp)
    nlam = kb.sb("nlam", [128, 1])
    kb.tt("dve", nlam, e[:, 3:4], e[:, 2:3], ALU.subtract)
    kb.ts("dve", nlam, nlam, -float(lam_init), ALU.add)
    qraw = kb.sb("qraw", [128, NTOK])
    kraw = kb.sb("kraw", [128, NTOK])
    qn = kb.sb("qn", [128, NTOK])
    kn = kb.sb("kn", [128, NTOK])
    k1m = kb.sb("k1m", [128, NTOK])
    k2m = kb.sb("k2m", [128, NTOK])
    vs = kb.sb("vs", [128, 18, 128])
    tmps = [kb.sb("tm%d" % i, [128, 512]) for i in range(4)]
    pts = [kb.sb("pt%d" % i, [128, 512]) for i in range(4)]
    fin = [kb.sb("fin%d" % i, [128, 512]) for i in range(4)]
    ob = kb.sb("ob", [128, 512])
    sqt = kb.sb("sqt", [128, 512])
    acc = cx.pp_tiles[0:4]
    sbk = cx.pp_tiles[4:8]
    si = 0
    pi = 0
    for h in range(NH):
        kb.dma("sp", qraw, qT[h])
        kb.dma("sp", kraw, kT[h])
        dma_r(kb, vs, vv[h].rearrange("(t p) d -> p t d", p=128))
        fm_rmsnorm(kb, cx, qraw, qn, bones, g[:, 0:1], 64, tmps, 128, NTOK)
        fm_rmsnorm(kb, cx, kraw, kn, bones, g[:, 1:2], 64, tmps, 128, NTOK)
        fm_rope(kb, cx, qn, rotT, tb[:, 0, :], tb[:, 1, :], tmps[2:4], 128, 256, 2048)
        fm_rope(kb, cx, kn, rotT, tb[:, 0, :], tb[:, 1, :], tmps[2:4], 128, 256, 2048)
        kb.ts("dve", k1m.v().r(), kn, g[:, 3:4], ALU.mult)
        kb.ts("dve", k2m.v().r(), kn, g[:, 4:5], ALU.mult)
        blocks = [(0, 256, 2)] + [(256 + 512 * i, 512, 18) for i in range(4)]
        for (c0, N, nkt) in blocks:
            for kt in range(nkt):
                for br, km in enumerate((k1m, k2m)):
                    ps_s = sbk[si % 4]
                    si += 1
                    kb.mm(ps_s[:, 0:N], km[:, kt * 128:(kt + 1) * 128], qn[:, c0:c0 + N])
                    pT = pts[pi % 4]
                    pi += 1
                    kb.act(pT[:, 0:N].r(), ps_s[:, 0:N], AF.Exp, scale=0.125)
                    kb.mm(acc[2 * br][:, 0:N], vs[:, kt, :], pT[:, 0:N], start=(kt == 0), stop=(kt == nkt - 1))
                    kb.mm(acc[2 * br + 1][:, 0:N], ones, pT[:, 0:N], start=(kt == 0), stop=(kt == nkt - 1))
            kb.recip(fin[0][:, 0:N], acc[1][:, 0:N])
            kb.tt("dve", fin[1][:, 0:N], acc[0][:, 0:N], fin[0][:, 0:N], ALU.mult)
            kb.recip(fin[2][:, 0:N], acc[3][:, 0:N])
            kb.tt("dve", fin[3][:, 0:N], acc[2][:, 0:N], fin[2][:, 0:N], ALU.mult)
            kb.stt(ob[:, 0:N], fin[3][:, 0:N], nlam, fin[1][:, 0:N], ALU.mult, ALU.add)
            kb.act(sqt[:, 0:N].r(), ob[:, 0:N], AF.Square)
            ps = sbk[si % 4]
            si += 1
            kb.mm(ps[:, 0:N], ones, sqt[:, 0:N])
            kb.act(fin[2][:, 0:N], ps[:, 0:N], AF.Sqrt, scale=1.0 / 128, bias=EPS)
            kb.recip(fin[2][:, 0:N], fin[2][:, 0:N])
            kb.stt(fin[1][:, 0:N], ob[:, 0:N], g[:, 2:3], fin[2][:, 0:N], ALU.mult, ALU.mult)
            kb.dma("sp", oT[h][:, c0:c0 + N], fin[1][:, 0:N])
    return kb.build()


def build_att1():
    kb = KB()
    NH = 8
    qnT = kb.dram("qnT", [NH, 128, 2048], "ExternalInput")
    qrT = kb.dram("qrT", [NH, 64, 2048], "ExternalInput")
    knT = kb.dram("knT", [NH, 128, NTOK], "ExternalInput")
    vv = kb.dram("v", [NH, NTOK, 128], "ExternalInput")
    krT = kb.dram("krT", [64, NTOK], "ExternalInput")
    cst = kb.dram("cst", [3, 128, 128], "ExternalInput")
    tabs = kb.dram("tabs", [2, 128, 2048], "ExternalInput")
    gv = kb.dram("gv", [128, 4], "ExternalInput")
    oT = kb.dram("oT", [NH, 128, 2048], "ExternalOutput")
    cx = Ctx(kb, 0)
    cs = kb.sb("cs", [128, 3, 128])
    dma_r(kb, cs, cst.rearrange("c p n -> p c n"))
    ones, rotT, B64 = cs[:, 0, :], cs[:, 1, :], cs[:, 2, :]
    tb = kb.sb("tb", [128, 2, 2048])
    kb.dma("sp", tb, tabs.rearrange("c p n -> p c n"))
    g = kb.sb("g", [128, 4])
    kb.dma("sp", g, gv)
    raw = kb.sb("raw", [128, NTOK])
    raw2 = kb.sb("raw2", [128, NTOK])
    kb.memset("dve", raw2, 0.0)
    qn = kb.sb("qn", [128, 2048])
    qr = kb.sb("qr", [128, 2048])
    kn = kb.sb("kn", [128, NTOK])
    krn = kb.sb("krn", [128, NTOK])
    vs = kb.sb("vs", [128, 18, 128])
    tmps = [kb.sb("tm%d" % i, [128, 512]) for i in range(4)]
    pts = [kb.sb("pt%d" % i, [128, 512]) for i in range(4)]
    fin = [kb.sb("fin%d" % i, [128, 512]) for i in range(3)]
    acc = cx.pp_tiles[0:2]
    sbk = cx.pp_tiles[2:5]
    cx.pp_tiles = cx.pp_tiles[5:8]
    si = 0
    pi = 0
    fi = 0
    scale = float((128 + 64) ** -0.5)
    kb.dma("sp", raw2[0:64, :], krT)
    fm_rmsnorm(kb, cx, raw2, krn, B64, g[:, 3:4], 64, tmps, 128, NTOK)
    fm_rope(kb, cx, krn, rotT, tb[:, 0, :], tb[:, 1, :], tmps[2:4], 128, 256, 2048)
    for h in range(NH):
        kb.dma("sp", raw[:, 0:2048], qnT[h])
        fm_rmsnorm(kb, cx, raw, qn, ones, g[:, 0:1], 128, tmps, 128, 2048)
        kb.dma("sp", raw2[0:64, 0:2048], qrT[h])
        fm_rmsnorm(kb, cx, raw2, qr, B64, g[:, 1:2], 64, tmps, 128, 2048)
        fm_rope(kb, cx, qr, rotT, tb[:, 0, :], tb[:, 1, :], tmps[2:4], 128, 0, 2048)
        kb.dma("sp", raw, knT[h])
        fm_rmsnorm(kb, cx, raw, kn, ones, g[:, 2:3], 128, tmps, 128, NTOK)
        dma_r(kb, vs, vv[h].rearrange("(t p) d -> p t d", p=128))
        for qb in range(4):
            c0 = qb * 512
            for kt in range(18):
                ps_s = sbk[si % 3]
                si += 1
                kb.mm(ps_s, kn[:, kt * 128:(kt + 1) * 128], qn[:, c0:c0 + 512], start=True, stop=False)
                kb.mm(ps_s, krn[:, kt * 128:(kt + 1) * 128], qr[:, c0:c0 + 512], start=False, stop=True)
                pT = pts[pi % 4]
                pi += 1
                kb.act(pT.v().r(), ps_s, AF.Exp, scale=scale)
                kb.mm(acc[0], vs[:, kt, :], pT, start=(kt == 0), stop=(kt == 17))
                kb.mm(acc[1], ones, pT, start=(kt == 0), stop=(kt == 17))
            f0 = fin[fi % 3]
            f1 = fin[(fi + 1) % 3]
            fi += 2
            kb.recip(f0, acc[1])
            kb.tt("dve", f1, acc[0], f0, ALU.mult)
            kb.dma("sp", oT[h][:, c0:c0 + 512], f1)
    return kb.build()


NCH = 36


def v3(view, n=64):
    return view.re(lambda a: a.rearrange("p (c n) -> p c n", n=n))


def bc_mid(view, c):
    return view.re(lambda a: a.unsqueeze(1).broadcast_to([a.shape[0], c, a.shape[1]]))


def bc_last(view, n):
    return view.re(lambda a: a.unsqueeze(2).broadcast_to([a.shape[0], a.shape[1], n]))


def build_gdn():
    kb = KB()
    NH = 4
    xin = [kb.dram(nm, [NH, 128, NTOK], "ExternalInput") for nm in ("xq", "xk", "xv")]
    z = kb.dram("z", [NH, 64, NCH, 128], "ExternalInput")
    gt = kb.dram("gt", [NH, 64, 4, NCH], "ExternalInput")
    cw = kb.dram("cw", [NH, 128, 3, 5], "ExternalInput")
    gp = kb.dram("gp", [NH, 64, 4], "ExternalInput")
    gon = kb.dram("gon", [64, 128], "ExternalInput")
    cst = kb.dram("cst", [6, 128, 128], "ExternalInput")
    o = kb.dram("o", [NH, 64, NCH, 128], "ExternalOutput")
    cx = Ctx(kb, 0)
    cs = kb.sb("cs", [128, 6, 128])
    kb.dma("sp", cs, cst.rearrange("c p n -> p c n"))
    ones = cs[:, 0, :]
    ones64 = cs[0:64, 0, 0:64]
    TRI = (cs[0:64, 1, 0:64], cs[0:64, 2, 0:64])
    NSTR_U, NSTR_L = cs[0:64, 3, 0:64], cs[0:64, 4, 0:64]
    I64, I128 = cs[0:64, 5, 0:64], cs[:, 5, :]
    gont = kb.sb("gont", [64, 128])
    kb.dma("sp", gont, gon)
    XW = NTOK + 8
    xpad = kb.sb("xpad", [128, XW])
    kb.memset("dve", xpad, 0.0)
    acc = kb.sb("acc", [128, NTOK + 4])
    qT_s = kb.sb("qT_s", [128, NTOK])
    kT_s = kb.sb("kT_s", [128, NTOK])
    qdT = kb.sb("qdT", [128, NTOK])
    nwT = kb.sb("nwT", [128, NTOK])
    k_tm = kb.sb("k_tm", [64, NCH, 128])
    v_tm = kb.sb("v_tm", [64, NCH, 128])
    o_tm = kb.sb("o_tm", [64, NCH, 128])
    z_tm = kb.sb("z_tm", [64, NCH, 128])
    TT_all = kb.sb("TT_all", [64, NCH, 64])
    qkT = kb.sb("qkT", [64, NCH, 64])
    tmps = [kb.sb("tm%d" % i, [128, 512]) for i in range(2)]
    gw = {nm: kb.sb(nm, [64, 512]) for nm in ("R", "Rb", "d", "gT", "gA", "kk", "Pa", "Pb", "PTa", "PTb", "Tm")}
    cwt = kb.sb("cwt", [128, 3, 5])
    gtt = kb.sb("gtt", [64, 4, NCH])
    gpt = kb.sb("gpt", [64, 4])
    gg = kb.sb("gg", [64, 2, NCH])
    beta = kb.sb("beta", [64, 2, NCH])
    sm = {nm: kb.sb(nm, [64, NCH]) for nm in ("e1", "sp", "Gcol", "kdc", "bxg", "tmpc", "ssq")}
    ea = kb.sb("ea", [64, 1])
    gtot = kb.sb("gtot", [128, NCH])
    sst = [kb.sb("s%d" % i, [128, 128]) for i in range(2)]
    small = {nm: [kb.sb("%s%d" % (nm, i), [64, 128]) for i in range(3)] for nm in ("vb", "kd", "vn", "kbt")}
    cnt = {"vb": 0, "kd": 0, "vn": 0, "kbt": 0}

    def nxt(nm):
        t = small[nm][cnt[nm] % 3]
        cnt[nm] += 1
        return t

    def chcols(ch):
        return slice(ch * 64, (ch + 1) * 64)

    def padcols(ch):
        c0 = 2 + 64 * ch if ch < 4 else 262 + 64 * (ch - 4)
        return slice(c0, c0 + 64)

    for h in range(NH):
        kb.dma("sp", cwt, cw[h])
        kb.dma("sp", gtt, gt[h])
        kb.dma("sp", gpt, gp[h])
        for ci in range(3):
            kb.dma("sp", xpad[:, 2:258], xin[ci][h][:, 0:256])
            kb.dma("sp", xpad[:, 262:2310], xin[ci][h][:, 256:NTOK])
            L = NTOK + 4
            kb.ts("dve", acc, xpad[:, 0:L], cwt[:, ci, 0:1], ALU.mult)
            for k in range(1, 5):
                kb.stt(acc, xpad[:, k:k + L], cwt[:, ci, k:k + 1], acc, ALU.mult, ALU.add)
            kb.act(xpad[:, 2:258], acc[:, 0:256], AF.Silu)
            kb.act(xpad[:, 262:2310], acc[:, 260:260 + 2048], AF.Silu)
            if ci < 2:
                dst = qT_s if ci == 0 else kT_s
                gain = float(128 ** -0.5) if ci == 0 else 1.0
                fm_rmsnorm(kb, cx, xpad[:, 2:258], dst[:, 0:256], ones, gain, 1, tmps, 128, 256, rr=False)
                fm_rmsnorm(kb, cx, xpad[:, 262:2310], dst[:, 256:NTOK], ones, gain, 1, tmps, 128, 2048, rr=False)
            if ci >= 1:
                dst = k_tm if ci == 1 else v_tm
                for c0 in range(0, NCH, 4):
                    ps = cx.pp()
                    for j in range(4):
                        ch = c0 + j
                        src = kT_s[:, chcols(ch)] if ci == 1 else xpad[:, padcols(ch)]
                        kb.tr(ps[0:64, j * 128:(j + 1) * 128], src, I128)
                    kb.copy("act" if (c0 // 4) % 2 else "dve", dst[:, c0:c0 + 4, :], v3(ps[0:64, :], 128))
        for di in range(2):
            kb.act(sm["e1"], gtt[:, 2 * di, :], AF.Exp, bias=gpt[:, 2 * di + 1:2 * di + 2])
            kb.act(sm["sp"], sm["e1"], AF.Ln, bias=1.0)
            kb.act(ea, gpt[:, 2 * di:2 * di + 1], AF.Exp)
            kb.ts("dve", gg[:, di, :], sm["sp"], ea, ALU.mult, -1.0, ALU.mult)
            kb.act(beta[:, di, :], gtt[:, 2 * di + 1, :], AF.Sigmoid)
        for di in range(2):
            fwd = di == 0
            tri = TRI[di]
            nstrM = NSTR_U if fwd else NSTR_L
            nstrA = NSTR_L if fwd else NSTR_U
            gdi = gg[:, di, :]
            bdi = beta[:, di, :]
            ps = cx.pp()
            kb.mm(ps[0:64, 0:NCH], tri, gdi, r=False)
            kb.copy("dve", sm["Gcol"], ps[0:64, 0:NCH])
            ps = cx.pp()
            kb.mm(ps[0:64, 0:NCH], ones64, gdi, r=False)
            kb.tt("dve", sm["tmpc"], ps[0:64, 0:NCH], sm["Gcol"], ALU.subtract)
            kb.act(sm["kdc"], sm["tmpc"], AF.Exp)
            ps = cx.pp()
            kb.mm(ps[:, 0:NCH], ones[0:64, :], gdi, r=False)
            kb.act(gtot, ps[:, 0:NCH], AF.Exp)
            kb.act(sm["tmpc"], sm["Gcol"], AF.Exp)
            kb.tt("dve", sm["bxg"], sm["tmpc"], bdi, ALU.mult)
            for g0 in range(0, NCH, 8):
                nchg = min(8, NCH - g0)
                W = nchg * 64
                cols = slice(g0 * 64, g0 * 64 + W)
                gs = slice(g0, g0 + nchg)
                w = {k: t[:, 0:W] for k, t in gw.items()}
                kb.tt("dve", v3(w["R"]), bc_mid(tri, nchg), bc_last(gdi[:, gs], 64), ALU.mult)
                psG = cx.pp()
                kb.mm(psG[:, 0:W], ones[0:64, :], w["R"], r=False)
                kb.act(qdT[:, cols], psG[:, 0:W], AF.Exp)
                kb.tt("dve", qdT[:, cols], qdT[:, cols], qT_s[:, cols], ALU.mult)
                kb.tt("dve", v3(w["d"]), v3(psG[0:64, 0:W]), bc_last(sm["Gcol"][:, gs], 64), ALU.subtract)
                kb.ts("dve", w["gT"], w["d"], 0.0, ALU.min)
                kb.act(w["gT"], w["gT"], AF.Exp)
                kb.tt("pool", v3(w["gT"]), v3(w["gT"]), bc_mid(tri, nchg), ALU.mult)
                kb.ts("dve", w["gA"], w["d"], 0.0, ALU.max)
                kb.act(w["gA"], w["gA"], AF.Exp, scale=-1.0)
                kb.tt("pool", v3(w["Rb"]), bc_mid(I64, nchg), bc_last(bdi[:, gs], 64), ALU.mult)
                psB = cx.pp()
                kb.mm(psB[0:64, 0:W], ones64, w["Rb"], r=False)
                psKK = cx.pp()
                for j in range(nchg):
                    ch = g0 + j
                    kb.mm(psKK[0:64, j * 64:(j + 1) * 64], kT_s[:, chcols(ch)], kT_s[:, chcols(ch)], r=False)
                kb.copy("act", w["kk"], psKK[0:64, 0:W])
                P, Pn, PT, PTn = w["Pa"], w["Pb"], w["PTa"], w["PTb"]
                kb.tt("dve", P, w["kk"], w["gT"], ALU.mult)
                kb.tt("dve", P, P, psB[0:64, 0:W], ALU.mult)
                kb.tt("pool", v3(P), v3(P), bc_mid(nstrM, nchg), ALU.mult)
                kb.tt("dve", PT, w["kk"], w["gA"], ALU.mult)
                kb.tt("dve", v3(PT), v3(PT), bc_last(bdi[:, gs], 64), ALU.mult)
                kb.tt("pool", v3(PT), v3(PT), bc_mid(nstrA, nchg), ALU.mult)
                psQK = cx.pp()
                for j in range(nchg):
                    ch = g0 + j
                    kb.mm(psQK[0:64, j * 64:(j + 1) * 64], kT_s[:, chcols(ch)], qT_s[:, chcols(ch)], r=False)
                kb.tt("dve", qkT[:, gs, :], v3(psQK[0:64, 0:W]), v3(w["gT"]), ALU.mult)
                TT = TT_all[:, gs, :]
                Tm = w["Tm"]
                kb.tt("dve", TT, v3(P), bc_mid(I64, nchg), ALU.add)
                kb.tt("pool", v3(Tm), v3(PT), bc_mid(I64, nchg), ALU.add)
                for it in range(5):
                    last = it == 4
                    psP = cx.pp()
                    for j in range(nchg):
                        kb.mm(psP[0:64, chcols(j)], PT[:, chcols(j)], P[:, chcols(j)], r=False)
                    if not last:
                        psPT = cx.pp()
                        for j in range(nchg):
                            kb.mm(psPT[0:64, chcols(j)], P[:, chcols(j)], PT[:, chcols(j)], r=False)
                    kb.copy("act", Pn, psP[0:64, 0:W])
                    if not last:
                        kb.copy("dve", PTn, psPT[0:64, 0:W])
                    psT = cx.pp()
                    for j in range(nchg):
                        kb.mm(psT[0:64, chcols(j)], Tm[:, chcols(j)], Pn[:, chcols(j)], r=False)
                    if not last:
                        psTm = cx.pp()
                        for j in range(nchg):
                            kb.mm(psTm[0:64, chcols(j)], Pn[:, chcols(j)], Tm[:, chcols(j)], r=False)
                    kb.tt("dve", TT, TT, v3(psT[0:64, 0:W]), ALU.add)
                    if not last:
                        kb.tt("dve", Tm, Tm, psTm[0:64, 0:W], ALU.add)
                    P, Pn = Pn, P
                    PT, PTn = PTn, PT
                psW = cx.pp()
                for j in range(nchg):
                    ch = g0 + j
                    kbt = nxt("kbt")
                    kb.ts("dve", kbt, k_tm[:, ch, :], sm["bxg"][:, ch:ch + 1], ALU.mult)
                    kb.mm(psW[:, chcols(j)], kbt, TT_all[:, ch, :], r=False)
                kb.ts("dve", nwT[:, cols], psW[:, 0:W], -1.0, ALU.mult)
            s, sn = sst[0], sst[1]
            kb.memset("pool", s, 0.0)
            order = list(range(NCH)) if fwd else [3, 2, 1, 0] + list(range(NCH - 1, 3, -1))
            for ch in order:
                vb = nxt("vb")
                kb.ts("dve", vb, v_tm[:, ch, :], bdi[:, ch:ch + 1], ALU.mult)
                kd = nxt("kd")
                kb.ts("dve", kd, k_tm[:, ch, :], sm["kdc"][:, ch:ch + 1], ALU.mult)
                ps_v = cx.pp()
                kb.mm(ps_v[0:64, 0:128], TT_all[:, ch, :], vb, start=True, stop=False, r=False)
                kb.mm(ps_v[0:64, 0:128], nwT[:, chcols(ch)], s, start=False, stop=True, r=False)
                vn = nxt("vn")
                kb.copy("act", vn, ps_v[0:64, 0:128])
                ps_o = cx.pp()
                kb.mm(ps_o[0:64, 0:128], qdT[:, chcols(ch)], s, start=True, stop=False, r=False)
                kb.mm(ps_o[0:64, 0:128], qkT[:, ch, :], vn, start=False, stop=True, r=False)
                if fwd:
                    kb.copy("act", o_tm[:, ch, :], ps_o[0:64, 0:128])
                else:
                    kb.tt("dve", o_tm[:, ch, :], o_tm[:, ch, :], ps_o[0:64, 0:128], ALU.add)
                ps_s = cx.pp()
                kb.mm(ps_s[:, 0:128], kd, vn, r=False)
                kb.stt(sn, s, gtot[:, ch:ch + 1], ps_s[:, 0:128], ALU.mult, ALU.add)
                s, sn = sn, s
        kb.dma("sp", z_tm, z[h])
        kb.tt("pool", k_tm, o_tm, o_tm, ALU.mult)
        kb.reduce(sm["ssq"], k_tm, ALU.add)
        kb.act(sm["tmpc"], sm["ssq"], AF.Sqrt, scale=1.0 / 128, bias=EPS)
        kb.recip(sm["tmpc"], sm["tmpc"])
        kb.tt("dve", k_tm, o_tm, bc_last(sm["tmpc"], 128), ALU.mult)
        kb.tt("pool", k_tm, k_tm, bc_mid(gont, NCH), ALU.mult)
        kb.act(z_tm, z_tm, AF.Silu)
        kb.tt("dve", z_tm, z_tm, k_tm, ALU.mult)
        kb.dma("sp", o[h], z_tm)
    return kb.build()


def run_gdn(proj, projc, P):
    tri_u = np.triu(np.ones((64, 64), np.float32))
    tri_l = np.tril(np.ones((64, 64), np.float32))
    c6 = np.zeros((6, 128, 128), np.float32)
    c6[0] = 1.0
    c6[1, :64, :64] = tri_u
    c6[2, :64, :64] = tri_l
    c6[3, :64, :64] = -np.triu(np.ones((64, 64), np.float32), 1)
    c6[4, :64, :64] = -np.tril(np.ones((64, 64), np.float32), -1)
    c6[5] = np.eye(128, dtype=np.float32)
    tm = lambda a: np.ascontiguousarray(a.reshape(NCH, 64, -1).transpose(1, 0, 2))
    ins = []
    for c in range(8):
        b, p = c // 2, c % 2
        cat = np.concatenate([projc[b], proj[b]], 0)
        hs = [4 * p + i for i in range(4)]
        fm = lambda off: np.ascontiguousarray(np.stack([cat[:, off + h * 128:off + (h + 1) * 128].T for h in hs]))
        gt = np.stack([np.stack([tm(cat[:, off + h:off + h + 1])[:, :, 0] for off in (7168, 7184, 7176, 7192)], 1) for h in hs])
        cw = np.stack([np.stack([P["conv"][:, off + h * 128:off + (h + 1) * 128].T for off in (0, 1024, 2048)], 1) for h in hs])
        gp = np.stack([np.stack([np.full(64, P[k][h], np.float32) for k in ("alog_f", "dtb_f", "alog_b", "dtb_b")], 1) for h in hs])
        ins.append({"xq": fm(3072), "xk": fm(4096), "xv": fm(5120),
                    "z": np.ascontiguousarray(np.stack([tm(cat[:, 6144 + h * 128:6144 + (h + 1) * 128]) for h in hs])),
                    "gt": np.ascontiguousarray(gt), "cw": np.ascontiguousarray(cw), "gp": np.ascontiguousarray(gp),
                    "gon": rep(P["o_norm"], 64), "cst": c6})
    res = run(build_gdn(), ins)
    o = np.zeros((4, NTOK, 1024), np.float32)
    for c in range(8):
        b, p = c // 2, c % 2
        for i in range(4):
            h = 4 * p + i
            o[b, :, h * 128:(h + 1) * 128] = res[c]["o"][i].transpose(1, 0, 2).reshape(NTOK, 128)
    return o[:, 256:], o[:, :256]


def build_pre1(T, groups):
    kb = KB()
    x = kb.dram("x", [T, D], "ExternalInput")
    vl = kb.dram("vl", [2, 128, D], "ExternalInput")
    vc = kb.dram("vc", [2, 128, D], "ExternalInput")
    n1 = kb.dram("n1", [128, D], "ExternalInput")
    w = kb.dram("w", [D, 1088], "ExternalInput")
    gqk = kb.dram("gqk", [2, 128, 512], "ExternalInput")
    wuq = kb.dram("wuq", [512, 3072], "ExternalInput")
    wukv = kb.dram("wukv", [512, 4096], "ExternalInput")
    idn = kb.dram("idn", [128, 128], "ExternalInput")
    q = kb.dram("q", [T, 3072], "ExternalOutput")
    kv = kb.dram("kv", [T, 4096], "ExternalOutput")
    kr = kb.dram("kr", [T, 64], "ExternalOutput")
    cx = Ctx(kb, 2, idn)
    ss, rt, rstd = small_scratch(kb)
    n1t = kb.sb("n1t", [128, D])
    kb.dma("sp", n1t, n1)
    gt = kb.sb("gt", [128, 2, 512])
    kb.dma("sp", gt, gqk.rearrange("c p n -> p c n"))
    vt = kb.sb("vt", [128, 2, D])
    xg = kb.sb("xg", [128, 3, D])
    hT = kb.sb("hT", [128, 16, 384])
    lT = kb.sb("lT", [128, 4, 384])
    pg = kb.sb("pg", [128, 3, 1088])
    htmp = kb.sb("htmp", [128, D])
    stg = [kb.sb("stg%d" % i, [128, 512]) for i in range(3)]
    si = 0
    for (t0, nt, is_ctx) in groups:
        kb.dma("sp", xg[:, 0:nt, :], x[t0 * 128:(t0 + nt) * 128, :].rearrange("(t p) d -> p t d", p=128))
        kb.dma("sp", vt, (vc if is_ctx else vl).rearrange("v p d -> p v d"))
        kb.stt(vt[:, 0, :], vt[:, 0, :], 1.0, n1t, ALU.add, ALU.mult)
        for t in range(nt):
            norm_rows(kb, xg[:, t, :], vt[:, 0, :], vt[:, 1, :], htmp, ss, rt, rstd, D)
            transpose_rows(kb, cx, htmp, hT, 16, t * 128)
        for (n0, nc_) in ((0, 512), (512, 512), (1024, 64)):
            W = cx.wload(w, 0, 16, n0, nc_)
            for t in range(nt):
                ps = cx.pp()
                for c in range(16):
                    kb.mm(ps[:, 0:nc_], hT[:, c, t * 128:(t + 1) * 128], W[:, c, :], start=(c == 0), stop=(c == 15))
                kb.copy("act" if t % 2 == 0 else "dve", pg[:, t, n0:n0 + nc_], ps[:, 0:nc_])
        kb.dma("sp", kr[t0 * 128:(t0 + nt) * 128, :].rearrange("(t p) d -> p t d", p=128), pg[:, 0:nt, 1024:1088])
        for (off, gi, wmat, ncols, outd) in ((0, 0, wuq, 3072, q), (512, 1, wukv, 4096, kv)):
            for t in range(nt):
                norm_rows(kb, pg[:, t, off:off + 512], gt[:, gi, :], None, htmp[:, 0:512], ss, rt, rstd, 512)
                transpose_rows(kb, cx, htmp, lT, 4, t * 128)
            for n0 in range(0, ncols, 512):
                W = cx.wload(wmat, 0, 4, n0, 512)
                for t in range(nt):
                    ps = cx.pp()
                    for c in range(4):
                        kb.mm(ps, lT[:, c, t * 128:(t + 1) * 128], W[:, c, :], start=(c == 0), stop=(c == 3))
                    s = stg[si % 3]
                    kb.copy("act" if si % 2 == 0 else "dve", s, ps)
                    si += 1
                    kb.dma("sp", outd[(t0 + t) * 128:(t0 + t + 1) * 128, n0:n0 + 512], s)
    return kb.build()


def run_pre1(x1, xc1, mod1, norm1, P):
    ins = []
    gqk = np.stack([rep(P["qa_norm"]), rep(P["kva_norm"])])
    for c in range(8):
        b, p = c // 2, c % 2
        vl, vc = modvecs(mod1, b, [1, 0])
        ins.append({"x": core_tokens(x1, xc1, b, p), "vl": vl, "vc": vc, "n1": rep(norm1), "w": P["w_in"],
                    "gqk": gqk, "wuq": P["w_uq"], "wukv": P["w_ukv"], "idn": IDN})
    res = run(build_pre1(1152, GROUPS0), ins)
    return (uncore_tokens([r["q"] for r in res], 3072), uncore_tokens([r["kv"] for r in res], 4096),
            uncore_tokens([r["kr"] for r in res], 64))


def run_att1(q, kv, kvc, kr, krc, P):
    cst = np.zeros((3, 128, 128), np.float32)
    cst[0] = 1.0
    cst[1, :64, :64] = rot_lhsT(64)
    cst[2, :64, :64] = 1.0
    cosT, sinT = rope_tables(64, 1)
    tabs = np.zeros((2, 128, 2048), np.float32)
    tabs[0, :64] = cosT
    tabs[1, :64] = sinT
    gv = np.zeros((128, 4), np.float32)
    gv[:, 0] = P["qn_norm"]
    gv[:64, 1] = P["qr_norm"]
    gv[:, 2] = P["kn_norm"]
    gv[:64, 3] = P["kr_norm"]
    ins = []
    for c in range(8):
        b, p = c // 2, c % 2
        hs = [8 * p + i for i in range(8)]
        kvcat = np.concatenate([kvc[b], kv[b]], 0)
        krcat = np.concatenate([krc[b], kr[b]], 0)
        ins.append({
            "qnT": np.ascontiguousarray(np.stack([q[b][:, h * 192:h * 192 + 128].T for h in hs])),
            "qrT": np.ascontiguousarray(np.stack([q[b][:, h * 192 + 128:(h + 1) * 192].T for h in hs])),
            "knT": np.ascontiguousarray(np.stack([kvcat[:, h * 256:h * 256 + 128].T for h in hs])),
            "v": np.ascontiguousarray(np.stack([kvcat[:, h * 256 + 128:(h + 1) * 256] for h in hs])),
            "krT": np.ascontiguousarray(krcat.T), "cst": cst, "tabs": tabs, "gv": gv})
    res = run(build_att1(), ins)
    o = np.zeros((4, 2048, D), np.float32)
    for c in range(8):
        b, p = c // 2, c % 2
        for i in range(8):
            h = 8 * p + i
            o[b, :, h * 128:(h + 1) * 128] = res[c]["oT"][i].T
    return o


def kernel(x, c, ctx, c_ctx, ada_w, ada_b, norm1, norm2, mlp_w1, mlp_w2,
           e_w_in, e_q_norm, e_k_norm, e_lam_q1, e_lam_k1, e_lam_q2, e_lam_k2, e_subln, e_conv,
           e_alog_f, e_alog_b, e_dtb_f, e_dtb_b, e_o_norm, e_w_out,
           o_w_in, o_qa_norm, o_w_uq, o_kva_norm, o_w_ukv, o_qn_norm, o_qr_norm, o_kn_norm, o_kr_norm,
           o_w_out):
    f = lambda a: np.ascontiguousarray(np.asarray(a, dtype=np.float32))
    x, c, ctx, c_ctx, ada_w, ada_b = f(x), f(c), f(ctx), f(c_ctx), f(ada_w), f(ada_b)
    norm1, norm2, mlp_w1, mlp_w2 = f(norm1), f(norm2), f(mlp_w1), f(mlp_w2)
    mod0, mod1 = run_mod(c, c_ctx, ada_w, ada_b)
    PE = dict(q_norm=f(e_q_norm)[0], k_norm=f(e_k_norm)[0], lam_q1=f(e_lam_q1)[0], lam_k1=f(e_lam_k1)[0],
              lam_q2=f(e_lam_q2)[0], lam_k2=f(e_lam_k2)[0], subln=f(e_subln)[0], conv=f(e_conv)[0],
              alog_f=f(e_alog_f)[0], alog_b=f(e_alog_b)[0], dtb_f=f(e_dtb_f)[0], dtb_b=f(e_dtb_b)[0],
              o_norm=f(e_o_norm)[0])
    proj, projc = run_pre0(x, ctx, mod0, norm1[0], f(e_w_in)[0])
    lam_init = 0.8 - 0.6 * math.exp(-0.3 * 0)
    oa, oca = run_att0(proj, projc, PE, lam_init)
    ob, ocb = run_gdn(proj, projc, PE)
    o = np.concatenate([oa, ob], -1)
    oc = np.concatenate([oca, ocb], -1)
    x1, xc1 = run_post(x, ctx, o, oc, mod0, norm2[0], f(e_w_out)[0], mlp_w1[0], mlp_w2[0], True)
    PO = dict(w_in=f(o_w_in)[0], qa_norm=f(o_qa_norm)[0], w_uq=f(o_w_uq)[0], kva_norm=f(o_kva_norm)[0],
              w_ukv=f(o_w_ukv)[0], qn_norm=f(o_qn_norm)[0], qr_norm=f(o_qr_norm)[0], kn_norm=f(o_kn_norm)[0],
              kr_norm=f(o_kr_norm)[0])
    (q, qc), (kv, kvc), (kr, krc) = run_pre1(x1, xc1, mod1, norm1[1], PO)
    o1 = run_att1(q, kv, kvc, kr, krc, PO)
    x2, _ = run_post(x1, None, o1, None, mod1, norm2[1], f(o_w_out)[0], mlp_w1[1], mlp_w2[1], False)
    return x2.astype(np.float32)


def rope_tables(rot_dim, reps):
    t = np.arange(2048)
    row = (t // 64).astype(np.float32)
    col = (t % 64).astype(np.float32)
    n_freq = rot_dim // 4
    inv_freq = (np.float32(10000.0) ** (-np.arange(n_freq, dtype=np.float32) / np.float32(n_freq))).astype(np.float32)
    ang = np.concatenate([row[:, None] * inv_freq, col[:, None] * inv_freq], -1).astype(np.float32)
    cos, sin = np.cos(ang).astype(np.float32), np.sin(ang).astype(np.float32)
    half = rot_dim // 2
    idx = np.arange(reps * rot_dim) % rot_dim % half
    return np.ascontiguousarray(cos[:, idx].T), np.ascontiguousarray(sin[:, idx].T)


def rot_lhsT(n, grp=64):
    h = grp // 2
    Rm = np.zeros((n, n), np.float32)
    for f in range(n):
        if f % grp < h:
            Rm[f, f + h] = -1.0
        else:
            Rm[f, f - h] = 1.0
    return np.ascontiguousarray(Rm.T)


def run_att0(proj, projc, P, lam_init):
    bones = np.kron(np.eye(2, dtype=np.float32), np.ones((64, 64), np.float32))
    cst = np.stack([bones, np.ones((128, 128), np.float32), rot_lhsT(128)])
    cosT, sinT = rope_tables(64, 2)
    tabs = np.stack([cosT, sinT])
    gv = np.zeros((128, 8), np.float32)
    gv[:, 0] = np.tile(P["q_norm"], 2)
    gv[:, 1] = np.tile(P["k_norm"], 2)
    gv[:, 2] = P["subln"]
    gv[0:64, 3] = 1.0
    gv[64:, 4] = 1.0
    lamv = np.stack([rep(P["lam_q1"]), rep(P["lam_k1"]), rep(P["lam_q2"]), rep(P["lam_k2"])], 1)
    ins = []
    for c in range(8):
        b, p = c // 2, c % 2
        cat = np.concatenate([projc[b], proj[b]], 0)
        hs = [4 * p + i for i in range(4)]
        ins.append({
            "qT": np.ascontiguousarray(np.stack([cat[:, h * 128:(h + 1) * 128].T for h in hs])),
            "kT": np.ascontiguousarray(np.stack([cat[:, 1024 + h * 128:1024 + (h + 1) * 128].T for h in hs])),
            "v": np.ascontiguousarray(np.stack([cat[:, 2048 + h * 128:2048 + (h + 1) * 128] for h in hs])),
            "cst": cst, "tabs": tabs, "gv": gv, "lamv": np.ascontiguousarray(lamv)})
    res = run(build_att0(lam_init), ins)
    o = np.zeros((4, NTOK, 1024), np.float32)
    for c in range(8):
        b, p = c // 2, c % 2
        for i in range(4):
            h = 4 * p + i
            o[b, :, h * 128:(h + 1) * 128] = res[c]["oT"][i].T
    return o[:, 256:], o[:, :256]
```
